# Optimizing a Trainium2 kernel written in Bass

```python
import math
import jax, jax.numpy as jnp
from jax import lax
import numpy as np

D_MODEL = 1024
BATCH = 8
SEQ = 8192
DEPTH = 1

D_MIX = 2 * D_MODEL
SSD_WIDTH = D_MIX // 2
SSD_HEAD_DIM = 64
SSD_HEADS = SSD_WIDTH // SSD_HEAD_DIM
SSD_GROUPS = 2
SSD_HPG = SSD_HEADS // SSD_GROUPS
SSD_STATE = 128
CONV_WIDTH = 4
RET_WIDTH = D_MIX - SSD_WIDTH
RET_HEADS = 8
RET_V_DIM = RET_WIDTH // RET_HEADS
RET_QK_DIM = RET_V_DIM // 2
CHUNK = 128
D_FF = -(-8 * D_MODEL // (3 * 256)) * 256
ROPE_BASE = 10000.0
EPS = 1e-6
DT_MIN = 0.001
DT_MAX = 0.1

BC_WIDTH = SSD_GROUPS * SSD_STATE
CONV_CH = SSD_WIDTH + 2 * BC_WIDTH
QK_WIDTH = RET_HEADS * RET_QK_DIM
PROJ_SIZES = [SSD_WIDTH, CONV_CH, SSD_HEADS, QK_WIDTH, QK_WIDTH, RET_WIDTH, RET_WIDTH]
PROJ_SPLITS = [int(s) for s in np.cumsum(PROJ_SIZES)[:-1]]
D_PROJ = sum(PROJ_SIZES)

kernel_name = "hybrid_ssd_retention_parallel_heads"


def rmsnorm(x, w):
    xf = x.astype(jnp.float32)
    y = xf * lax.rsqrt(jnp.mean(xf * xf, axis=-1, keepdims=True) + EPS)
    return (y * w.astype(jnp.float32)).astype(x.dtype)


def causal_depthwise_conv(u, w, b):
    ch = u.shape[-1]
    out = lax.conv_general_dilated(
        u, w[:, None, :].astype(u.dtype), window_strides=(1,),
        padding=[(CONV_WIDTH - 1, 0)],
        dimension_numbers=("NWC", "WIO", "NWC"),
        feature_group_count=ch)
    return out + b.astype(u.dtype)


def chunk_state_scan(states, decay):
    def step(carry, inp):
        s, d = inp
        return (carry * d + s).astype(carry.dtype), carry
    _, prev = lax.scan(step, jnp.zeros_like(states[0]), (states, decay))
    return prev


def ssd_mixer(z, xbc, dt_raw, conv_w, conv_b, dt_bias, a_log, d_skip, norm_w):
    bsz, seqlen, _ = z.shape
    nc = seqlen // CHUNK
    xbc = jax.nn.silu(causal_depthwise_conv(xbc, conv_w, conv_b))
    xs, bm, cm = jnp.split(xbc, [SSD_WIDTH, SSD_WIDTH + BC_WIDTH], axis=-1)
    x_c = xs.reshape(bsz, nc, CHUNK, SSD_GROUPS, SSD_HPG, SSD_HEAD_DIM)
    b_c = bm.reshape(bsz, nc, CHUNK, SSD_GROUPS, SSD_STATE)
    c_c = cm.reshape(bsz, nc, CHUNK, SSD_GROUPS, SSD_STATE)
    dt = jax.nn.softplus(dt_raw.astype(jnp.float32) + dt_bias.astype(jnp.float32))
    dt_c = dt.reshape(bsz, nc, CHUNK, SSD_GROUPS, SSD_HPG)
    a = -jnp.exp(a_log.astype(jnp.float32)).reshape(SSD_GROUPS, SSD_HPG)
    a_cs = jnp.cumsum(dt_c * a, axis=2)
    xdt = x_c.astype(jnp.float32) * dt_c[..., None]

    a_cs_t = jnp.moveaxis(a_cs, 2, -1)
    seg = a_cs_t[..., :, None] - a_cs_t[..., None, :]
    mask = jnp.tril(jnp.ones((CHUNK, CHUNK), dtype=bool))
    decay = jnp.exp(jnp.where(mask, seg, -jnp.inf))
    cb = jnp.einsum("bclgn,bcsgn->bcgls", c_c, b_c).astype(jnp.float32)
    y_diag = jnp.einsum("bcghls,bcsghp->bclghp", cb[:, :, :, None] * decay, xdt)

    decay_to_end = jnp.exp(a_cs[:, :, -1:] - a_cs)
    states = jnp.einsum("bclgn,bclgh,bclghp->bcghpn",
                        b_c.astype(jnp.float32), decay_to_end, xdt)
    chunk_decay = jnp.exp(a_cs[:, :, -1])
    prev = chunk_state_scan(jnp.moveaxis(states, 1, 0),
                            jnp.moveaxis(chunk_decay, 1, 0)[..., None, None])
    prev = jnp.moveaxis(prev, 0, 1)
    y_off = jnp.einsum("bclgn,bcghpn,bclgh->bclghp",
                       c_c.astype(jnp.float32), prev, jnp.exp(a_cs))

    d = d_skip.astype(jnp.float32).reshape(SSD_GROUPS, SSD_HPG, 1)
    y = y_diag + y_off + x_c.astype(jnp.float32) * d
    y = y.reshape(bsz, seqlen, SSD_WIDTH) * jax.nn.silu(z.astype(jnp.float32))
    yg = y.reshape(bsz, seqlen, SSD_GROUPS, SSD_WIDTH // SSD_GROUPS)
    yg = yg * lax.rsqrt(jnp.mean(yg * yg, axis=-1, keepdims=True) + EPS)
    y = yg.reshape(bsz, seqlen, SSD_WIDTH) * norm_w.astype(jnp.float32)
    return y.astype(z.dtype)


def rotary(t, positions):
    half = t.shape[-1] // 2
    inv_freq = ROPE_BASE ** (-jnp.arange(half, dtype=jnp.float32) / half)
    ang = positions[:, None] * inv_freq[None, :]
    cos = jnp.cos(ang)[:, None, :]
    sin = jnp.sin(ang)[:, None, :]
    tf = t.astype(jnp.float32)
    t1, t2 = tf[..., :half], tf[..., half:]
    return jnp.concatenate([t1 * cos - t2 * sin, t1 * sin + t2 * cos], axis=-1)


def retention_mixer(q, k, v, g, norm_w):
    bsz, seqlen, _ = q.shape
    nc = seqlen // CHUNK
    positions = jnp.arange(seqlen, dtype=jnp.float32)
    q = rotary(q.reshape(bsz, seqlen, RET_HEADS, RET_QK_DIM), positions)
    k = rotary(k.reshape(bsz, seqlen, RET_HEADS, RET_QK_DIM), positions) * (RET_QK_DIM ** -0.5)
    v = v.astype(jnp.float32).reshape(bsz, seqlen, RET_HEADS, RET_V_DIM)
    qc = q.reshape(bsz, nc, CHUNK, RET_HEADS, RET_QK_DIM)
    kc = k.reshape(bsz, nc, CHUNK, RET_HEADS, RET_QK_DIM)
    vc = v.reshape(bsz, nc, CHUNK, RET_HEADS, RET_V_DIM)

    log_gamma = jnp.log1p(-jnp.exp2(-5.0 - jnp.arange(RET_HEADS, dtype=jnp.float32)))
    pos = jnp.arange(CHUNK, dtype=jnp.float32)
    diff = pos[:, None] - pos[None, :]
    intra_decay = jnp.where(diff[None] >= 0,
                            jnp.exp(jnp.maximum(diff, 0.0)[None] * log_gamma[:, None, None]),
                            0.0)

    scores = jnp.einsum("bclhd,bcshd->bchls", qc, kc) * intra_decay
    y_intra = jnp.einsum("bchls,bcshv->bclhv", scores, vc)

    k_decay = jnp.exp((CHUNK - 1 - pos)[:, None] * log_gamma[None, :])
    states = jnp.einsum("bclhd,lh,bclhv->bchdv", kc, k_decay, vc)
    chunk_decay = jnp.exp(CHUNK * log_gamma)[None, None, :, None, None]
    chunk_decay = jnp.broadcast_to(chunk_decay, (nc, 1, RET_HEADS, 1, 1))
    prev = chunk_state_scan(jnp.moveaxis(states, 1, 0), chunk_decay)
    prev = jnp.moveaxis(prev, 0, 1)
    q_decay = jnp.exp((pos + 1.0)[:, None] * log_gamma[None, :])
    y_inter = jnp.einsum("bclhd,bchdv,lh->bclhv", qc, prev, q_decay)

    y = (y_intra + y_inter).reshape(bsz, seqlen, RET_HEADS, RET_V_DIM)
    mu = jnp.mean(y, axis=-1, keepdims=True)
    var = jnp.mean(jnp.square(y - mu), axis=-1, keepdims=True)
    y = ((y - mu) * lax.rsqrt(var + EPS)).reshape(bsz, seqlen, RET_WIDTH)
    y = y * norm_w.astype(jnp.float32) * jax.nn.silu(g.astype(jnp.float32))
    return y.astype(g.dtype)


def setup_inputs(seed: int = 0) -> dict:
    key = jax.random.key(seed)
    ks = jax.random.split(key, 17)
    f32 = jnp.float32

    def nrm(k, shape, scale):
        return jax.random.normal(k, shape, f32) * scale

    def gain(k, n):
        return 1.0 + 0.01 * jax.random.normal(k, (DEPTH, n), f32)

    dt = jnp.exp(jax.random.uniform(ks[4], (DEPTH, SSD_HEADS), f32,
                                    math.log(DT_MIN), math.log(DT_MAX)))
    dt_bias = dt + jnp.log(-jnp.expm1(-dt))
    a_log = jnp.log(jax.random.uniform(ks[5], (DEPTH, SSD_HEADS), f32, 1.0, 16.0))
    return {
        "x": jax.random.normal(ks[0], (BATCH, SEQ, D_MODEL), f32),
        "norm1_w": gain(ks[1], D_MODEL),
        "w_in": nrm(ks[2], (DEPTH, D_MODEL, D_PROJ), D_MODEL ** -0.5),
        "conv_w": nrm(ks[3], (DEPTH, CONV_WIDTH, CONV_CH), CONV_WIDTH ** -0.5),
        "conv_b": nrm(ks[6], (DEPTH, CONV_CH), 0.01),
        "dt_bias": dt_bias,
        "a_log": a_log,
        "d_skip": gain(ks[7], SSD_HEADS),
        "ssd_norm_w": gain(ks[8], SSD_WIDTH),
        "ret_norm_w": gain(ks[9], RET_WIDTH),
        "w_out": nrm(ks[10], (DEPTH, D_MIX, D_MODEL), D_MIX ** -0.5),
        "norm2_w": gain(ks[11], D_MODEL),
        "w_gate": nrm(ks[12], (DEPTH, D_MODEL, D_FF), D_MODEL ** -0.5),
        "w_up": nrm(ks[13], (DEPTH, D_MODEL, D_FF), D_MODEL ** -0.5),
        "w_down": nrm(ks[14], (DEPTH, D_FF, D_MODEL), D_FF ** -0.5),
        "final_norm_w": 1.0 + 0.01 * jax.random.normal(ks[15], (D_MODEL,), f32),
    }


def reference(x, norm1_w, w_in, conv_w, conv_b, dt_bias, a_log, d_skip, ssd_norm_w,
              ret_norm_w, w_out, norm2_w, w_gate, w_up, w_down, final_norm_w):
    for i in range(DEPTH):
        h = rmsnorm(x, norm1_w[i])
        proj = jnp.einsum("bsd,de->bse", h, w_in[i])
        z, xbc, dt_raw, q, k, v, g = jnp.split(proj, PROJ_SPLITS, axis=-1)
        y_ssd = ssd_mixer(z, xbc, dt_raw, conv_w[i], conv_b[i], dt_bias[i], a_log[i],
                          d_skip[i], ssd_norm_w[i])
        y_ret = retention_mixer(q, k, v, g, ret_norm_w[i])
        mixed = jnp.concatenate([y_ssd, y_ret], axis=-1)
        x = x + jnp.einsum("bse,ed->bsd", mixed, w_out[i])
        h = rmsnorm(x, norm2_w[i])
        a = jax.nn.silu(jnp.einsum("bsd,df->bsf", h, w_gate[i]))
        u = jnp.einsum("bsd,df->bsf", h, w_up[i])
        x = x + jnp.einsum("bsf,fd->bsd", a * u, w_down[i])
    return rmsnorm(x, final_norm_w)
```

```python
import numpy as np
import ml_dtypes
from contextlib import ExitStack
import concourse.bass as bass
import concourse.mybir as mybir
from concourse.bass_utils import run_bass_kernel_spmd

F32 = mybir.dt.float32
BF16 = mybir.dt.bfloat16
AF = mybir.ActivationFunctionType
ALU = mybir.AluOpType
AX = mybir.AxisListType

D = 1024
DPROJ = 5648
DFF = 2816
NF = DFF // 128
EPS = 1e-6
SEQ = 8192
NCORES = 8
O_Z, O_XBC, O_DT, O_Q, O_K, O_V, O_G = 0, 1024, 2560, 2576, 3088, 3600, 4624


class T:
    def __init__(self, ap, key):
        self.ap = ap
        self.key = key

    def __getitem__(self, idx):
        return self.ap[idx]


class Sched:
    CAP = 30000

    def __init__(self, nc, es):
        self.nc, self.es = nc, es
        self.eng = {"pe": nc.tensor, "dve": nc.vector, "act": nc.scalar,
                    "pool": nc.gpsimd, "sp": nc.sync}
        self.ops, self.pending = [], []
        self.lastw, self.readers = {}, {}
        self.sig_count = {}
        self.sig_sems = {}
        self.dma_sems = {}
        self.waited = {e: {} for e in self.eng}
        self.nsem = 0

    def _newsem(self):
        self.nsem += 1
        return self.es.enter_context(self.nc.semaphore("s%d" % self.nsem))

    def op(self, eng, fn, reads=(), writes=(), dma_key=None):
        rec = dict(id=len(self.ops), eng=eng, fn=fn, deps=set(), sig=None,
                   need=False, dma_key=dma_key)
        wkeys = [t.key for t in writes]
        for t in reads:
            if t.key in self.lastw:
                rec["deps"].add(self.lastw[t.key])
        for k in wkeys:
            if k in self.lastw:
                rec["deps"].add(self.lastw[k])
            for r in self.readers.get(k, ()):
                rec["deps"].add(r)
        for k in wkeys:
            self.lastw[k] = rec["id"]
            self.readers[k] = []
        for t in reads:
            if t.key not in wkeys:
                self.readers.setdefault(t.key, []).append(rec["id"])
        rec["deps"].discard(rec["id"])
        for d in rec["deps"]:
            p = self.ops[d]
            if self._pe_pair(p, rec):
                continue
            p["need"] = True
        rec["lab"] = (eng, [t.key for t in reads], wkeys)
        self.ops.append(rec)
        self.pending.append(rec)
        return rec

    @staticmethod
    def _pe_pair(p, c):
        return (p["eng"] == "pe" and c["eng"] == "pe"
                and p["dma_key"] is None and c["dma_key"] is None)

    def do(self, eng, meth, reads, writes, *a, **kw):
        h = self.eng[eng]
        return self.op(eng, lambda: getattr(h, meth)(*a, **kw), reads, writes)

    def group(self, eng, calls, reads, writes):
        h = self.eng[eng]

        def fn():
            ins = None
            for meth, a, kw in calls:
                ins = getattr(h, meth)(*a, **kw)
            return ins
        return self.op(eng, fn, reads, writes)

    def dma(self, eng, key, pairs, reads, writes):
        h = self.eng[eng]

        def fn(sem):
            for out, in_ in pairs:
                h.dma_start(out=out, in_=in_).then_inc(sem, 16)
            return len(pairs)
        return self.op(eng, fn, reads, writes, dma_key=key)

    def _wait(self, eng, sig):
        sem, val, sid = sig
        w = self.waited[eng]
        if w.get(sid, 0) >= val:
            return
        self.eng[eng].wait_ge(sem, val)
        if getattr(self, "_dbg", False):
            print("   WAIT", eng, sid, val, flush=True)
        w[sid] = val

    def flush(self):
        import os
        self._nfl = getattr(self, "_nfl", 0) + 1
        kc = int(os.environ.get("KCUT", "0")) if self._nfl == 1 else int(os.environ.get("KCUT2", "0"))
        if kc:
            print("PENDING", len(self.pending), flush=True)
            for i, r in enumerate(self.pending):
                print("OP", i, r["lab"], flush=True)
            self.pending = self.pending[:kc]
        for _i, rec in enumerate(self.pending):
            self._dbg = (self._nfl >= 2 and 128 <= _i <= 141 and bool(kc))
            if self._dbg:
                print("EMIT", rec["id"], rec["lab"], sorted(rec["deps"]), flush=True)
            need = {}
            own = None
            for d in sorted(rec["deps"]):
                p = self.ops[d]
                if p["sig"] is None or self._pe_pair(p, rec):
                    continue
                sem, val, sid = p["sig"]
                if p["eng"] == rec["eng"] and p["dma_key"] is None:
                    if own is None or val > own[1] or sid != own[2]:
                        own = p["sig"]
                elif sid not in need or need[sid][1] < val:
                    need[sid] = p["sig"]
            wl = [need[sid] for sid in need] + ([own] if own is not None else [])
            e_ = rec["eng"]
            wl = [g for g in wl if self.waited[e_].get(g[2], 0) < g[1]]
            for g in wl:
                self.waited[e_][g[2]] = g[1]
            embed = None
            for g in wl:
                self.eng[e_].wait_ge(g[0], g[1])
            if rec["dma_key"] is not None:
                k = rec["dma_key"]
                if k not in self.dma_sems:
                    self.dma_sems[k] = [self._newsem(), 0, "d" + k]
                ent = self.dma_sems[k]
                n = rec["fn"](ent[0])
                ent[1] += 16 * n
                rec["sig"] = (ent[0], ent[1], ent[2])
            else:
                ins = rec["fn"]()
                if embed is not None:
                    ins._wait_ge(embed[0], embed[1])
                if rec["need"]:
                    e = rec["eng"]
                    cnt = self.sig_count.get(e, 0)
                    if cnt % self.CAP == 0:
                        self.sig_sems[e] = (self._newsem(), "%s%d" % (e, cnt // self.CAP))
                    sem, sid = self.sig_sems[e]
                    ins.then_inc(sem, 1)
                    self.sig_count[e] = cnt + 1
                    rec["sig"] = (sem, cnt % self.CAP + 1, sid)
                    if self._dbg:
                        print("   SIG", rec["sig"][1:], flush=True)
        self.pending = []

    def dummy(self, e):
        t = self.scr[e]
        if e == "dve":
            return self.nc.vector.tensor_copy(out=t[:, 0:1], in_=t[:, 1:2])
        if e == "act":
            return self.nc.scalar.copy(out=t[:, 0:1], in_=t[:, 1:2])
        return self.nc.gpsimd.tensor_copy(out=t[:, 0:1], in_=t[:, 1:2])

    def barrier(self):
        self.flush()
        marks = []
        for e, h in self.eng.items():
            sem = self._newsem()
            h.drain().then_inc(sem, 1)
            marks.append((sem, 1, "b%d" % self.nsem))
        for e in self.eng:
            for m in marks:
                self._wait(e, m)
            for k, ent in self.dma_sems.items():
                if ent[1] > 0:
                    self._wait(e, (ent[0], ent[1], ent[2]))
        for rec in self.ops:
            rec["sig"] = None
        self.lastw, self.readers = {}, {}


class Ctx:
    pass


def _build(L, part=0):
    import os
    STOP = int(os.environ.get('KSTOP', '99'))
    NCH = L // 128 if STOP > 1 else 0
    if os.environ.get('KSKIP1'):
        NCH = 0
    nc = bass.Bass("TRN2", target_bir_lowering=False)

    def din(name, shape, dt=F32):
        if (part == 1 and name not in P1_KEYS) or (part == 2 and name not in P2_KEYS):
            return None
        return nc.dram_tensor(name, list(shape), dt, kind="ExternalInput").ap()

    x_d = din("x", [L, D])
    win_d = din("w_in", [D, DPROJ])
    wout_d = din("w_out", [2048, D])
    wg_d = din("w_gate", [D, DFF])
    wu_d = din("w_up", [D, DFF])
    wd_d = din("w_down", [DFF, D])
    n1w_d = din("n1w", [128, 8])
    n2w_d = din("n2w", [128, 8])
    mixw_d = din("mixw", [128, 16])
    fw_d = din("fw", [128, D])
    cw_d = din("cw", [128, 48])
    cb_d = din("cb", [128, 12])
    cbrow_d = din("cbrow", [1, 1536])
    dtb_d = din("dtb", [128, 16])
    alog_d = din("alog", [128, 16])
    dsk_d = din("dsk", [128, 16])
    ident_d = din("ident", [128, 128])
    U_d = din("U", [128, 128])
    Ls_d = din("Ls", [128, 128])
    dq_d = din("dq", [128, 1024])
    ks_d = din("ks", [128, 8])
    g128_d = din("g128", [128, 8])
    cs_d = din("cs", [L, 128])
    if part != 1:
        out_d = nc.dram_tensor("out", [L, D], F32, kind="ExternalOutput").ap()
    mx_d = nc.dram_tensor("mx", [L, 2048], mybir.dt.uint16,
                          kind={0: "Internal", 1: "ExternalOutput", 2: "ExternalInput"}[part]).ap()

    with ExitStack() as es:
        S = Sched(nc, es)
        S.scr = {e: es.enter_context(nc.sbuf_tensor("scr_" + e, [128, 2], F32)) for e in ("dve", "act", "pool")}
        for e in ("dve", "act", "pool"):
            S.eng[e].memset(S.scr[e][:], 0.0) if e != "act" else nc.vector.memset(S.scr[e][:], 0.0)
        psb = [T(es.enter_context(nc.psum_tensor("ps%d" % i, [128, 512], F32)), "ps%d" % i)
               for i in range(8)]
        pstate = {"i": 0}

        def psnext():
            t = psb[pstate["i"] % 8]
            pstate["i"] += 1
            return t

        def h4(ap, h):
            return ap.rearrange("p (h d) -> p h d", h=h)

        if part != 2:
            with ExitStack() as e1:
                def sb(name, shape, dt=F32):
                    return T(e1.enter_context(nc.sbuf_tensor("a_" + name, list(shape), dt)), name)

                W = sb("W_in", [128, 8, DPROJ], BF16)
                ident = sb("ident", [128, 128], BF16)
                U = sb("U", [128, 128])
                Ls = sb("Ls", [128, 128])
                ones = sb("ones", [128, 128])
                onesrow = sb("onesrow", [1, 128], BF16)
                cbrow = sb("cbrow", [1, 1536], BF16)
                dq = sb("dq", [128, 8, 128], BF16)
                ks = sb("ks", [128, 8])
                g128 = sb("g128", [128, 8])
                cw = sb("cw", [128, 48])
                cb = sb("cb", [128, 12])
                dtb = sb("dtb", [128, 16])
                abc = sb("abc", [128, 16])
                dsk = sb("dsk", [128, 16])
                n1w = sb("n1w", [128, 8])
                dg = sb("dg", [128, 48, 128], BF16)
                dD = sb("dD", [128, 16, 128], BF16)
                xs_r = [sb("xs%d" % i, [128, D]) for i in range(2)]
                cs_r = [sb("cs%d" % i, [128, 128]) for i in range(2)]
                junk = sb("junk", [128, D], BF16)
                ss = sb("ss", [128, 1]); ss2 = sb("ss2", [128, 1]); rstd = sb("rstd", [128, 1])
                xn = sb("xn", [128, D], BF16)
                hT = sb("hT", [128, 8, 128], BF16)
                usb = sb("usb", [128, 12, 131], BF16)
                x_bf = sb("x_bf", [128, D], BF16)
                Bfm = sb("Bfm", [128, 2, 128], BF16)
                Cfm = sb("Cfm", [128, 2, 128], BF16)
                Btm = sb("Btm", [128, 256], BF16)
                sz = sb("sz", [128, D], BF16)
                sg = sb("sg", [128, D], BF16)
                v_bf = sb("v_bf", [128, D], BF16)
                t0 = sb("t0", [128, 16]); e1t = sb("e1t", [128, 16]); dt = sb("dt", [128, 16])
                dtA = sb("dtA", [128, 16]); acs = sb("acs", [128, 16]); ea = sb("ea", [128, 16])
                cd = sb("cd", [128, 16]); t1 = sb("t1", [128, 16]); dte = sb("dte", [128, 16])
                wgt = sb("wgt", [128, 16])
                xdt = sb("xdt", [128, D], BF16)
                xw = sb("xw", [128, D], BF16)
                rhsh = sb("rhsh", [128, 8, 128])
                decT = sb("decT", [128, 8, 128], BF16)
                CBm = sb("CBm", [128, 2, 128], BF16)
                MT = sb("MT", [128, 16, 128], BF16)
                yo = sb("yo", [128, D])
                y = sb("y", [128, D])
                ssg = sb("ssg", [128, 2]); ssg2 = sb("ssg2", [128, 2]); rstdg = sb("rstdg", [128, 2])
                mixed_r = [sb("mixed%d" % i, [128, 2048], BF16) for i in range(2)]
                St = sb("St", [128, D]); S_bf = sb("S_bf", [128, D], BF16)
                rA = sb("rA", [128, 512]); rB = sb("rB", [128, 512])
                q_rot = sb("q_rot", [128, 512], BF16)
                kp = sb("kp", [128, 512], BF16)
                qT = sb("qT", [128, 8, 128], BF16)
                kT = sb("kT", [64, 8, 128], BF16)
                PT = sb("PT", [128, 8, 128], BF16)
                Rt = sb("Rt", [64, D]); R_bf = sb("R_bf", [128, D], BF16)
                s1 = sb("s1", [128, 8]); s2 = sb("s2", [128, 8]); mean = sb("mean", [128, 8])
                msq = sb("msq", [128, 8]); var = sb("var", [128, 8]); lnv = sb("lnv", [128, 8])
                rstdr = sb("rstdr", [128, 8]); nmr = sb("nmr", [128, 8])
                stage = [yo, y, rA, rB]

                S.dma("sp", "c0", [(U[:], U_d[:, :]), (Ls[:], Ls_d[:, :]),
                                   (ks[:], ks_d[:, :]), (g128[:], g128_d[:, :]),
                                   (cw[:], cw_d[:, :]), (cb[:], cb_d[:, :]), (dtb[:], dtb_d[:, :]),
                                   (abc[:], alog_d[:, :]), (dsk[:], dsk_d[:, :]), (n1w[:], n1w_d[:, :])],
                      [], [U, Ls, ks, g128, cw, cb, dtb, abc, dsk, n1w])
                S.dma("pool", "c1", [(cbrow[:], cbrow_d[:, :]), (ident[:], ident_d[:, :]),
                                     (dq[:].rearrange("p h l -> p (h l)"), dq_d[:, :])], [], [cbrow, ident, dq])
                S.do("pool", "memset", [], [ones], ones[:], 1.0)
                S.do("pool", "memset", [], [onesrow], onesrow[:], 1.0)
                S.do("pool", "memset", [], [usb], usb[:], 0.0)
                S.do("pool", "memset", [], [St], St[:], 0.0)
                S.do("pool", "memset", [], [S_bf], S_bf[:], 0.0)
                S.do("pool", "memset", [], [Rt], Rt[:], 0.0)
                S.do("pool", "memset", [], [R_bf], R_bf[:], 0.0)
                S.do("pool", "memset", [], [qT], qT[:], 0.0)
                S.do("act", "activation", [abc], [abc], out=abc[:], in_=abc[:], func=AF.Exp)
                S.do("dve", "tensor_scalar_mul", [abc], [abc], out=abc[:], in0=abc[:], scalar1=-1.0)
                for j in range(12):
                    for k in range(4):
                        S.do("dve" if (j + k) % 2 else "pool", "tensor_scalar_mul", [ident, cw], [dg],
                             out=dg[:, j * 4 + k, :], in0=ident[:], scalar1=cw[:, j * 4 + k:j * 4 + k + 1])
                for h in range(16):
                    S.do("dve" if h % 2 else "pool", "tensor_scalar_mul", [ident, dsk], [dD],
                         out=dD[:, h, :], in0=ident[:], scalar1=dsk[:, h:h + 1])
                pi = 0
                for k in range(8):
                    c0 = 0
                    while c0 < DPROJ:
                        st = stage[pi % 4]
                        wmax = 1024 if pi % 4 < 2 else 512
                        n = min(wmax, DPROJ - c0)
                        S.dma("sp", "wl%d" % (pi % 4), [(st[:, 0:n], win_d[k * 128:(k + 1) * 128, c0:c0 + n])],
                              [], [st])
                        if pi % 2 == 0:
                            S.do("dve", "tensor_scalar_mul", [st, n1w], [W], out=W[:, k, c0:c0 + n],
                                 in0=st[:, 0:n], scalar1=n1w[:, k:k + 1])
                        else:
                            S.do("act", "mul", [st, n1w], [W], out=W[:, k, c0:c0 + n],
                                 in_=st[:, 0:n], mul=n1w[:, k:k + 1])
                        c0 += n
                        pi += 1

                for c in range(NCH):
                    r0 = c * 128
                    xs = xs_r[c % 2]
                    cs = cs_r[c % 2]
                    mixed = mixed_r[c % 2]
                    if c == 0:
                        S.dma("sp", "x0", [(xs[:], x_d[0:128, :])], [], [xs])
                        S.dma("sp", "cs0", [(cs[:], cs_d[0:128, :])], [], [cs])
                    if c + 1 < NCH:
                        n_ = (c + 1) % 2
                        S.dma("sp", "x%d" % n_, [(xs_r[n_][:], x_d[r0 + 128:r0 + 256, :])], [], [xs_r[n_]])
                        S.dma("sp", "cs%d" % n_, [(cs_r[n_][:], cs_d[r0 + 128:r0 + 256, :])], [], [cs_r[n_]])
                    S.do("act", "activation", [xs], [junk, ss], out=junk[:], in_=xs[:], func=AF.Square,
                         accum_out=ss[:])
                    S.do("act", "activation", [ss], [ss2], out=ss2[:], in_=ss[:], func=AF.Ln,
                         scale=1.0 / D, bias=EPS)
                    S.do("act", "activation", [ss2], [rstd], out=rstd[:], in_=ss2[:], func=AF.Exp, scale=-0.5)
                    S.do("dve", "tensor_scalar_mul", [xs, rstd], [xn], out=xn[:], in0=xs[:], scalar1=rstd[:, 0:1])
                    for hf in range(2):
                        p = psnext()
                        S.group("pe", [("matmul", (p[:, i * 128:(i + 1) * 128],),
                                        dict(lhsT=xn[:, (hf * 4 + i) * 128:(hf * 4 + i + 1) * 128],
                                             rhs=ident[:], start=True, stop=True)) for i in range(4)],
                                [xn, ident], [p])
                        S.do("dve" if hf else "act", "tensor_copy" if hf else "copy", [p], [hT],
                             out=hT[:, hf * 4:(hf + 1) * 4, :].rearrange("p k t -> p (k t)"), in_=p[:])
                    def tm_block(col0, n):
                        pp = psnext()
                        S.group("pe", [("matmul", (pp[:, 0:n],),
                                        dict(lhsT=hT[:, k, :], rhs=W[:, k, col0:col0 + n],
                                             start=(k == 0), stop=(k == 7))) for k in range(8)],
                                [hT, W], [pp])
                        return pp
                    pd = tm_block(O_DT, 16)
                    S.do("dve", "tensor_tensor", [pd, dtb], [t0], out=t0[:], in0=pd[:, 0:16], in1=dtb[:], op=ALU.add)
                    S.do("act", "activation", [t0], [e1t], out=e1t[:], in_=t0[:], func=AF.Exp)
                    S.do("act", "activation", [e1t], [dt], out=dt[:], in_=e1t[:], func=AF.Ln, bias=1.0)
                    S.do("dve", "tensor_tensor", [dt, abc], [dtA], out=dtA[:], in0=dt[:], in1=abc[:], op=ALU.mult)
                    pa = psnext()
                    S.group("pe", [("matmul", (pa[:, 0:16],), dict(lhsT=U[:], rhs=dtA[:], start=True, stop=True)),
                                   ("matmul", (pa[:, 16:32],), dict(lhsT=ones[:], rhs=dtA[:], start=True, stop=True))],
                            [U, ones, dtA], [pa])
                    S.do("dve", "tensor_copy", [pa], [acs], out=acs[:], in_=pa[:, 0:16])
                    S.do("act", "activation", [pa], [ea], out=ea[:], in_=pa[:, 0:16], func=AF.Exp)
                    S.do("act", "activation", [pa], [cd], out=cd[:], in_=pa[:, 16:32], func=AF.Exp)
                    S.do("dve", "tensor_tensor", [pa, acs], [t1], out=t1[:], in0=pa[:, 16:32], in1=acs[:],
                         op=ALU.subtract)
                    S.do("act", "activation", [t1], [dte], out=dte[:], in_=t1[:], func=AF.Exp)
                    S.do("dve", "tensor_tensor", [dte, dt], [wgt], out=wgt[:], in0=dte[:], in1=dt[:], op=ALU.mult)
                    S.do("pool", "tensor_copy", [usb], [usb], out=usb[:, :, 0:3], in_=usb[:, :, 128:131])
                    for b in range(3):
                        p = psnext()
                        calls = []
                        for i in range(4):
                            j = b * 4 + i
                            for k in range(8):
                                calls.append(("matmul", (p[:, i * 128:(i + 1) * 128],),
                                              dict(lhsT=W[:, k, O_XBC + j * 128:O_XBC + (j + 1) * 128],
                                                   rhs=hT[:, k, :], start=(k == 0), stop=(k == 7))))
                        S.group("pe", calls, [W, hT], [p])
                        S.do("act" if b % 2 else "dve", "copy" if b % 2 else "tensor_copy", [p], [usb],
                             out=usb[:, b * 4:(b + 1) * 4, 3:131], in_=h4(p[:], 4))
                    for which in range(2):
                        pp = tm_block(O_Q if which == 0 else O_K, 512)
                        p3 = h4(pp[:], 8)
                        S.do("dve", "tensor_tensor", [pp, cs], [rA], out=h4(rA[:], 8), in0=p3,
                             in1=cs[:, 0:64].unsqueeze(1).to_broadcast([128, 8, 64]), op=ALU.mult)
                        S.do("dve", "tensor_tensor", [pp, cs], [rB], out=h4(rB[:], 8)[:, :, 0:32], in0=p3[:, :, 32:64],
                             in1=cs[:, 64:96].unsqueeze(1).to_broadcast([128, 8, 32]), op=ALU.mult)
                        S.do("dve", "tensor_tensor", [pp, cs], [rB], out=h4(rB[:], 8)[:, :, 32:64], in0=p3[:, :, 0:32],
                             in1=cs[:, 96:128].unsqueeze(1).to_broadcast([128, 8, 32]), op=ALU.mult)
                        if which == 0:
                            S.do("pool", "tensor_tensor", [rA, rB], [q_rot], out=q_rot[:], in0=rA[:], in1=rB[:], op=ALU.add)
                        else:
                            S.do("pool", "tensor_tensor", [rA, rB], [rA], out=rA[:], in0=rA[:], in1=rB[:], op=ALU.add)
                            S.do("pool", "tensor_tensor", [rA, ks], [kp], out=h4(kp[:], 8), in0=h4(rA[:], 8),
                                 in1=ks[:].unsqueeze(2).to_broadcast([128, 8, 64]), op=ALU.mult)
                    for hf in range(2):
                        p = psnext()
                        calls = []
                        for i in range(4):
                            j = hf * 4 + i
                            o = p[:, i * 128:(i + 1) * 128]
                            for k in range(4):
                                calls.append(("matmul", (o,), dict(lhsT=usb[:, j, k:k + 128],
                                                                   rhs=dg[:, j * 4 + k, :], start=(k == 0), stop=False)))
                            calls.append(("matmul", (o,), dict(lhsT=onesrow[0:1, :],
                                                               rhs=cbrow[0:1, j * 128:(j + 1) * 128],
                                                               start=False, stop=True)))
                        S.group("pe", calls, [usb, dg, onesrow, cbrow], [p])
                        S.do("act", "activation", [p], [x_bf], out=x_bf[:, hf * 512:(hf + 1) * 512], in_=p[:],
                             func=AF.Silu)
                    p = psnext()
                    calls = []
                    for i in range(4):
                        j = 8 + i
                        for k in range(4):
                            calls.append(("matmul", (p[:, i * 128:(i + 1) * 128],),
                                          dict(lhsT=dg[:, j * 4 + k, :], rhs=usb[:, j, k:k + 128],
                                               start=(k == 0), stop=(k == 3))))
                    S.group("pe", calls, [usb, dg], [p])
                    for i in range(4):
                        dst = Bfm if i < 2 else Cfm
                        S.do("act", "activation", [p, cb], [dst], out=dst[:, i % 2, :],
                             in_=p[:, i * 128:(i + 1) * 128], func=AF.Silu, bias=cb[:, 8 + i:9 + i])
                    p = psnext()
                    calls = []
                    for i in range(2):
                        j = 8 + i
                        o = p[:, i * 128:(i + 1) * 128]
                        for k in range(4):
                            calls.append(("matmul", (o,), dict(lhsT=usb[:, j, k:k + 128], rhs=dg[:, j * 4 + k, :],
                                                               start=(k == 0), stop=False)))
                        calls.append(("matmul", (o,), dict(lhsT=onesrow[0:1, :],
                                                           rhs=cbrow[0:1, j * 128:(j + 1) * 128],
                                                           start=False, stop=True)))
                    S.group("pe", calls, [usb, dg, onesrow, cbrow], [p])
                    S.do("act", "activation", [p], [Btm], out=Btm[:], in_=p[:, 0:256], func=AF.Silu)

                    for hf in range(2):
                        pp = tm_block(O_Z + hf * 512, 512)
                        S.do("act", "activation", [pp], [sz], out=sz[:, hf * 512:(hf + 1) * 512], in_=pp[:],
                             func=AF.Silu)
                    for hf in range(2):
                        pp = tm_block(O_G + hf * 512, 512)
                        S.do("act", "activation", [pp], [sg], out=sg[:, hf * 512:(hf + 1) * 512], in_=pp[:],
                             func=AF.Silu)
                    for hf in range(2):
                        pp = tm_block(O_V + hf * 512, 512)
                        S.do("dve", "tensor_copy", [pp], [v_bf], out=v_bf[:, hf * 512:(hf + 1) * 512], in_=pp[:])
                    S.do("dve", "tensor_tensor", [x_bf, dt], [xdt], out=h4(xdt[:], 16), in0=h4(x_bf[:], 16),
                         in1=dt[:].unsqueeze(2).to_broadcast([128, 16, 64]), op=ALU.mult)
                    S.do("pool", "tensor_tensor", [x_bf, wgt], [xw], out=h4(xw[:], 16), in0=h4(x_bf[:], 16),
                         in1=wgt[:].unsqueeze(2).to_broadcast([128, 16, 64]), op=ALU.mult)
                    pcb = psnext()
                    S.group("pe", [("matmul", (pcb[:, g * 128:(g + 1) * 128],),
                                    dict(lhsT=Bfm[:, g, :], rhs=Cfm[:, g, :], start=True, stop=True))
                                   for g in range(2)], [Bfm, Cfm], [pcb])
                    S.do("dve", "tensor_tensor", [pcb, U], [CBm], out=CBm[:],
                         in0=pcb[:, 0:256].rearrange("p (g l) -> p g l", g=2),
                         in1=U[:].unsqueeze(1).to_broadcast([128, 2, 128]), op=ALU.mult)
                    for g in range(2):
                        S.do("pool", "tensor_tensor", [U, dtA], [rhsh], out=rhsh[:],
                             in0=U[:].unsqueeze(1).to_broadcast([128, 8, 128]),
                             in1=dtA[:, g * 8:(g + 1) * 8].unsqueeze(2).to_broadcast([128, 8, 128]), op=ALU.mult)
                        for q4 in range(2):
                            pg = psnext()
                            S.group("pe", [("matmul", (pg[:],),
                                            dict(lhsT=Ls[:], rhs=rhsh[:, q4 * 4:(q4 + 1) * 4, :].rearrange("p h l -> p (h l)"),
                                                 start=True, stop=True))], [Ls, rhsh], [pg])
                            S.do("act", "activation", [pg], [decT],
                                 out=decT[:, q4 * 4:(q4 + 1) * 4, :].rearrange("p h l -> p (h l)"), in_=pg[:], func=AF.Exp)
                        S.do("dve", "tensor_tensor", [decT, CBm], [MT], out=MT[:, g * 8:(g + 1) * 8, :], in0=decT[:],
                             in1=CBm[:, g:g + 1, :].to_broadcast([128, 8, 128]), op=ALU.mult)
                    for g in range(2):
                        py = psnext()
                        calls = []
                        for hh in range(8):
                            h = g * 8 + hh
                            o = py[:, hh * 64:(hh + 1) * 64]
                            calls.append(("matmul", (o,), dict(lhsT=MT[:, h, :], rhs=xdt[:, h * 64:(h + 1) * 64],
                                                               start=True, stop=False)))
                            calls.append(("matmul", (o,), dict(lhsT=dD[:, h, :], rhs=x_bf[:, h * 64:(h + 1) * 64],
                                                               start=False, stop=True)))
                        S.group("pe", calls, [MT, xdt, dD, x_bf], [py])
                        po = psnext()
                        S.group("pe", [("matmul", (po[:],), dict(lhsT=Cfm[:, g, :], rhs=S_bf[:, g * 512:(g + 1) * 512],
                                                                 start=True, stop=True))], [Cfm, S_bf], [po])
                        S.do("dve", "tensor_tensor", [po, ea], [yo], out=h4(yo[:, g * 512:(g + 1) * 512], 8),
                             in0=h4(po[:], 8), in1=ea[:, g * 8:(g + 1) * 8].unsqueeze(2).to_broadcast([128, 8, 64]),
                             op=ALU.mult)
                        S.do("dve", "tensor_tensor", [py, yo], [y], out=y[:, g * 512:(g + 1) * 512], in0=py[:],
                             in1=yo[:, g * 512:(g + 1) * 512], op=ALU.add)
                    S.do("pool", "tensor_tensor", [y, sz], [y], out=y[:], in0=y[:], in1=sz[:], op=ALU.mult)
                    for g in range(2):
                        S.do("act", "activation", [y], [junk, ssg], out=junk[:, 0:512], in_=y[:, g * 512:(g + 1) * 512],
                             func=AF.Square, accum_out=ssg[:, g:g + 1])
                    S.do("act", "activation", [ssg], [ssg2], out=ssg2[:], in_=ssg[:], func=AF.Ln, scale=1.0 / 512, bias=EPS)
                    S.do("act", "activation", [ssg2], [rstdg], out=rstdg[:], in_=ssg2[:], func=AF.Exp, scale=-0.5)
                    for g in range(2):
                        S.do("dve", "tensor_scalar_mul", [y, rstdg], [mixed], out=mixed[:, g * 512:(g + 1) * 512],
                             in0=y[:, g * 512:(g + 1) * 512], scalar1=rstdg[:, g:g + 1])
                    S.do("pool", "tensor_tensor", [St, cd], [St], out=h4(St[:], 16), in0=h4(St[:], 16),
                         in1=cd[:].unsqueeze(2).to_broadcast([128, 16, 64]), op=ALU.mult)
                    for g in range(2):
                        pst = psnext()
                        S.group("pe", [("matmul", (pst[:],), dict(lhsT=Btm[:, g * 128:(g + 1) * 128],
                                                                  rhs=xw[:, g * 512:(g + 1) * 512], start=True, stop=True))],
                                [Btm, xw], [pst])
                        S.do("dve", "tensor_tensor", [pst, St], [St], out=St[:, g * 512:(g + 1) * 512], in0=pst[:],
                             in1=St[:, g * 512:(g + 1) * 512], op=ALU.add)
                    S.do("act", "copy", [St], [S_bf], out=S_bf[:], in_=St[:])

                    for which in range(2):
                        src = q_rot if which == 0 else kp
                        dst = qT if which == 0 else kT
                        for hf in range(2):
                            pt = psnext()
                            calls = []
                            for i in range(4):
                                h = hf * 4 + i
                                calls.append(("matmul", (pt[0:64, i * 128:(i + 1) * 128],),
                                              dict(lhsT=src[:, h * 64:(h + 1) * 64],
                                                   rhs=(dq[:, h, :] if which == 0 else ident[:]),
                                                   start=True, stop=True)))
                            S.group("pe", calls, [src, dq, ident], [pt])
                            S.do("act" if hf else "dve", "copy" if hf else "tensor_copy", [pt], [dst],
                                 out=dst[0:64, hf * 4:(hf + 1) * 4, :], in_=h4(pt[0:64, :], 4))
                    for hf in range(2):
                        psc = psnext()
                        S.group("pe", [("matmul", (psc[:, i * 128:(i + 1) * 128],),
                                        dict(lhsT=kT[:, hf * 4 + i, :], rhs=qT[0:64, hf * 4 + i, :], start=True, stop=True))
                                       for i in range(4)], [kT, qT], [psc])
                        S.do("dve", "tensor_tensor", [psc, U], [PT], out=PT[:, hf * 4:(hf + 1) * 4, :], in0=h4(psc[:], 4),
                             in1=U[:].unsqueeze(1).to_broadcast([128, 4, 128]), op=ALU.mult)
                    pyr = []
                    for hf in range(2):
                        pr = psnext()
                        calls = []
                        for i in range(4):
                            h = hf * 4 + i
                            o = pr[:, i * 128:(i + 1) * 128]
                            calls.append(("matmul", (o,), dict(lhsT=PT[:, h, :], rhs=v_bf[:, h * 128:(h + 1) * 128],
                                                               start=True, stop=False)))
                            calls.append(("matmul", (o,), dict(lhsT=qT[:, h, :], rhs=R_bf[:, h * 128:(h + 1) * 128],
                                                               start=False, stop=True)))
                        S.group("pe", calls, [PT, v_bf, qT, R_bf], [pr])
                        pyr.append(pr)
                        S.do("act", "copy", [pr], [y], out=y[:, hf * 512:(hf + 1) * 512], in_=pr[:])
                        S.do("dve", "tensor_reduce", [y], [s1], out=s1[:, hf * 4:(hf + 1) * 4],
                             in_=h4(y[:, hf * 512:(hf + 1) * 512], 4), axis=AX.X, op=ALU.add)
                        S.do("act", "activation", [pr], [yo], out=yo[:, hf * 512:(hf + 1) * 512], in_=pr[:], func=AF.Square)
                        S.do("dve", "tensor_reduce", [yo], [s2], out=s2[:, hf * 4:(hf + 1) * 4],
                             in_=h4(yo[:, hf * 512:(hf + 1) * 512], 4), axis=AX.X, op=ALU.add)
                    S.do("dve", "tensor_scalar_mul", [s1], [mean], out=mean[:], in0=s1[:], scalar1=1.0 / 128)
                    S.do("dve", "tensor_tensor", [mean], [msq], out=msq[:], in0=mean[:], in1=mean[:], op=ALU.mult)
                    S.do("dve", "tensor_scalar_mul", [s2], [s2], out=s2[:], in0=s2[:], scalar1=1.0 / 128)
                    S.do("dve", "tensor_tensor", [s2, msq], [var], out=var[:], in0=s2[:], in1=msq[:], op=ALU.subtract)
                    S.do("act", "activation", [var], [lnv], out=lnv[:], in_=var[:], func=AF.Ln, bias=EPS)
                    S.do("act", "activation", [lnv], [rstdr], out=rstdr[:], in_=lnv[:], func=AF.Exp, scale=-0.5)
                    S.do("dve", "tensor_tensor", [mean, rstdr], [nmr], out=nmr[:], in0=mean[:], in1=rstdr[:], op=ALU.mult)
                    S.do("dve", "tensor_scalar_mul", [nmr], [nmr], out=nmr[:], in0=nmr[:], scalar1=-1.0)
                    for hf in range(2):
                        pr = pyr[hf]
                        ysl = y[:, hf * 512:(hf + 1) * 512]
                        S.do("dve", "tensor_tensor", [y, rstdr], [y], out=h4(ysl, 4), in0=h4(ysl, 4),
                             in1=rstdr[:, hf * 4:(hf + 1) * 4].unsqueeze(2).to_broadcast([128, 4, 128]), op=ALU.mult)
                        S.do("pool", "tensor_tensor", [y, nmr], [y], out=h4(ysl, 4), in0=h4(ysl, 4),
                             in1=nmr[:, hf * 4:(hf + 1) * 4].unsqueeze(2).to_broadcast([128, 4, 128]), op=ALU.add)
                        S.do("pool", "tensor_tensor", [y, sg], [mixed], out=mixed[:, 1024 + hf * 512:1024 + (hf + 1) * 512],
                             in0=ysl, in1=sg[:, hf * 512:(hf + 1) * 512], op=ALU.mult)
                    for hf in range(2):
                        prs = psnext()
                        S.group("pe", [("matmul", (prs[0:64, i * 128:(i + 1) * 128],),
                                        dict(lhsT=kp[:, (hf * 4 + i) * 64:(hf * 4 + i + 1) * 64],
                                             rhs=v_bf[:, (hf * 4 + i) * 128:(hf * 4 + i + 1) * 128], start=True, stop=True))
                                       for i in range(4)], [kp, v_bf], [prs])
                        rsl = Rt[:, hf * 512:(hf + 1) * 512]
                        S.do("dve", "tensor_tensor", [prs, Rt], [Rt], out=rsl, in0=prs[0:64, :], in1=rsl, op=ALU.add)
                        S.do("pool", "tensor_tensor", [Rt, g128], [Rt], out=h4(rsl, 4), in0=h4(rsl, 4),
                             in1=g128[0:64, hf * 4:(hf + 1) * 4].unsqueeze(2).to_broadcast([64, 4, 128]), op=ALU.mult)
                    S.do("act", "copy", [Rt], [R_bf], out=R_bf[0:64, :], in_=Rt[:])
                    S.dma("sp", "mx%d" % (c % 2), [(mx_d[r0:r0 + 128, :], mixed[:].bitcast(mybir.dt.uint16))], [mixed], [])
                S.barrier()

        if STOP < 3 or part == 1:
            return nc
        TP = 256 if L % 256 == 0 else 128
        NT = TP // 128
        hist = {"i": 0}

        def ps_hi():
            t = psb[hist["i"] % 4]
            hist["i"] += 1
            return t
        with ExitStack() as e2:
            def sb(name, shape, dt=F32):
                return T(e2.enter_context(nc.sbuf_tensor("b_" + name, list(shape), dt)), name)

            Wo = sb("Wo", [128, 16, D], BF16)
            Wg = sb("Wg", [128, 8, DFF], BF16)
            Wu = sb("Wu", [128, 8, DFF], BF16)
            Wd = sb("Wd", [128, NF, D], BF16)
            ident = sb("ident2", [128, 128], BF16)
            n2w = sb("n2w", [128, 8])
            mixw = sb("mixw", [128, 16])
            fw = sb("fw", [128, D])
            x1 = [sb("x1_%d" % i, [128, D]) for i in range(NT)]
            mxt = sb("mxt", [128, 2048], BF16)
            mxT = sb("mxT", [128, 16, 128], BF16)
            mxTq = [T(mxT.ap, "mxT.q%d" % i) for i in range(4)]
            junk = sb("junk2", [128, D], BF16)
            ss = sb("ss_", [128, 1]); ss2 = sb("ss2_", [128, 1]); rstd = sb("rstd_", [128, 1])
            xn = sb("xn2", [128, D], BF16)
            h2T = sb("h2T", [128, 8, TP], BF16)
            sgt = [sb("sgt%d" % i, [128, TP]) for i in range(2)]
            act = [sb("act%d" % i, [128, TP], BF16) for i in range(2)]
            ot = [sb("ot%d" % i, [128, D]) for i in range(2)]
            stage = ot + x1

            S.dma("sp", "c2", [(n2w[:], n2w_d[:, :]), (mixw[:], mixw_d[:, :]),
                               (fw[:], fw_d[:, :])], [], [n2w, mixw, fw])
            S.dma("pool", "wd", [(ident[:], ident_d[:, :])]
                  + [(Wd[:, f, :], wd_d[f * 128:(f + 1) * 128, :]) for f in range(NF)], [], [ident, Wd])
            pi = 0
            jobs = [(Wo, wout_d, k, 0, D, mixw) for k in range(16)]
            for k in range(8):
                for c0 in range(0, DFF, 1024):
                    jobs.append((Wg, wg_d, k, c0, min(1024, DFF - c0), n2w))
                    jobs.append((Wu, wu_d, k, c0, min(1024, DFF - c0), n2w))
            ns = len(stage)
            for (Wt, src, k, c0, n, gain) in jobs:
                st = stage[pi % ns]
                S.dma("sp", "wl2_%d" % (pi % ns), [(st[:, 0:n], src[k * 128:(k + 1) * 128, c0:c0 + n])], [], [st])
                if pi % 2 == 0:
                    S.do("dve", "tensor_scalar_mul", [st, gain], [Wt], out=Wt[:, k, c0:c0 + n], in0=st[:, 0:n],
                         scalar1=gain[:, k:k + 1])
                else:
                    S.do("act", "mul", [st, gain], [Wt], out=Wt[:, k, c0:c0 + n], in_=st[:, 0:n],
                         mul=gain[:, k:k + 1])
                pi += 1

            for s in range(L // TP):
                for ti in range(NT):
                    r0 = s * TP + ti * 128
                    xt = x1[ti]
                    S.dma("sp", "xb%d" % ti, [(xt[:], x_d[r0:r0 + 128, :])], [], [xt])
                    S.dma("sp", "mxl", [(mxt[:].bitcast(mybir.dt.uint16), mx_d[r0:r0 + 128, :])], [], [mxt])
                    pq = [ps_hi() for _ in range(4)]
                    S.group("pe", [("matmul", (pq[qd][:, i * 128:(i + 1) * 128],),
                                    dict(lhsT=mxt[:, (qd * 4 + i) * 128:(qd * 4 + i + 1) * 128], rhs=ident[:],
                                         start=True, stop=True)) for qd in range(4) for i in range(4)],
                            [mxt, ident], pq)
                    for qd in range(4):
                        S.do("act" if qd % 2 else "dve", "copy" if qd % 2 else "tensor_copy", [pq[qd]], [mxTq[qd]],
                             out=mxT[:, qd * 4:(qd + 1) * 4, :].rearrange("p k t -> p (k t)"), in_=pq[qd][:])
                    for hf in range(2):
                        p = ps_hi()
                        S.group("pe", [("matmul", (p[:],), dict(lhsT=mxT[:, k, :], rhs=Wo[:, k, hf * 512:(hf + 1) * 512],
                                                                start=(k == 0), stop=(k == 15))) for k in range(16)],
                                mxTq + [Wo], [p])
                        S.do("dve", "tensor_tensor", [p, xt], [xt], out=xt[:, hf * 512:(hf + 1) * 512], in0=p[:],
                             in1=xt[:, hf * 512:(hf + 1) * 512], op=ALU.add)
                    S.do("act", "activation", [xt], [junk, ss], out=junk[:], in_=xt[:], func=AF.Square, accum_out=ss[:])
                    S.do("act", "activation", [ss], [ss2], out=ss2[:], in_=ss[:], func=AF.Ln, scale=1.0 / D, bias=EPS)
                    S.do("act", "activation", [ss2], [rstd], out=rstd[:], in_=ss2[:], func=AF.Exp, scale=-0.5)
                    S.do("dve", "tensor_scalar_mul", [xt, rstd], [xn], out=xn[:], in0=xt[:], scalar1=rstd[:, 0:1])
                    for hf in range(2):
                        p = ps_hi()
                        S.group("pe", [("matmul", (p[:, i * 128:(i + 1) * 128],),
                                        dict(lhsT=xn[:, (hf * 4 + i) * 128:(hf * 4 + i + 1) * 128], rhs=ident[:],
                                             start=True, stop=True)) for i in range(4)], [xn, ident], [p])
                        S.do("act" if hf else "dve", "copy" if hf else "tensor_copy", [p], [h2T],
                             out=h2T[:, hf * 4:(hf + 1) * 4, ti * 128:(ti + 1) * 128], in_=h4(p[:], 4))
                pdn = [[psb[4 + ti * 2 + hf] for hf in range(2)] for ti in range(NT)]
                prev = None
                for f in range(NF + 1):
                    if f < NF:
                        pgt = ps_hi()
                        S.group("pe", [("matmul", (pgt[:, 0:TP],), dict(lhsT=Wg[:, k, f * 128:(f + 1) * 128], rhs=h2T[:, k, :],
                                                                         start=(k == 0), stop=(k == 7))) for k in range(8)]
                                + [("matmul", (pgt[:, 256:256 + TP],), dict(lhsT=Wu[:, k, f * 128:(f + 1) * 128], rhs=h2T[:, k, :],
                                                                             start=(k == 0), stop=(k == 7))) for k in range(8)],
                                [Wg, Wu, h2T], [pgt])
                        sgf = sgt[f % 2]
                        af = act[f % 2]
                        S.do("act", "activation", [pgt], [sgf], out=sgf[:], in_=pgt[:, 0:TP], func=AF.Silu)
                        S.do("dve", "tensor_tensor", [pgt, sgf], [af], out=af[:], in0=pgt[:, 256:256 + TP], in1=sgf[:],
                             op=ALU.mult)
                    if prev is not None:
                        fp, ap_ = prev
                        for ti in range(NT):
                            for hf in range(2):
                                S.group("pe", [("matmul", (pdn[ti][hf][:],),
                                                dict(lhsT=ap_[:, ti * 128:(ti + 1) * 128], rhs=Wd[:, fp, hf * 512:(hf + 1) * 512],
                                                     start=(fp == 0), stop=(fp == NF - 1)))], [ap_, Wd], [pdn[ti][hf]])
                    prev = (f, act[f % 2]) if f < NF else None
                for ti in range(NT):
                    r0 = s * TP + ti * 128
                    xt = x1[ti]
                    o = ot[(s * NT + ti) % 2]
                    for hf in range(2):
                        S.do("dve", "tensor_tensor", [pdn[ti][hf], xt], [xt], out=xt[:, hf * 512:(hf + 1) * 512],
                             in0=pdn[ti][hf][:], in1=xt[:, hf * 512:(hf + 1) * 512], op=ALU.add)
                    S.do("act", "activation", [xt], [junk, ss], out=junk[:], in_=xt[:], func=AF.Square, accum_out=ss[:])
                    S.do("act", "activation", [ss], [ss2], out=ss2[:], in_=ss[:], func=AF.Ln, scale=1.0 / D, bias=EPS)
                    S.do("act", "activation", [ss2], [rstd], out=rstd[:], in_=ss2[:], func=AF.Exp, scale=-0.5)
                    S.do("dve", "tensor_scalar_mul", [xt, rstd], [xt], out=xt[:], in0=xt[:], scalar1=rstd[:, 0:1])
                    S.do("dve", "tensor_tensor", [xt, fw], [o], out=o[:], in0=xt[:], in1=fw[:], op=ALU.mult)
                    S.dma("sp", "o%d" % ((s * NT + ti) % 2), [(out_d[r0:r0 + 128, :], o[:])], [o], [])
            S.barrier()
    return nc


def _host_consts(L):
    l = np.arange(128, dtype=np.float64)
    U = (l[:, None] <= l[None, :]).astype(np.float32)
    Ls = (l[:, None] > l[None, :]).astype(np.float32)
    lg = np.log1p(-np.exp2(-5.0 - np.arange(8, dtype=np.float64)))
    dqv = np.exp((l[:, None] + 1.0) * lg[None, :])
    dq = np.zeros((128, 8, 128), np.float32)
    for h in range(8):
        dq[np.arange(128), h, np.arange(128)] = dqv[:, h]
    ks = (np.exp(-(l[:, None] + 1.0) * lg[None, :]) * (64 ** -0.5)).astype(np.float32)
    g128 = np.broadcast_to(np.exp(128.0 * lg)[None, :], (128, 8)).astype(np.float32)
    pos = np.arange(L, dtype=np.float32)
    inv = (10000.0 ** (-np.arange(32, dtype=np.float32) / 32)).astype(np.float32)
    ang = (pos[:, None] * inv[None, :]).astype(np.float32)
    cos, sin = np.cos(ang).astype(np.float32), np.sin(ang).astype(np.float32)
    cs = np.concatenate([cos, cos, -sin, sin], axis=1).astype(np.float32)
    return dict(ident=np.eye(128, dtype=np.float32), U=U, Ls=Ls, dq=dq.reshape(128, 1024),
                ks=ks, g128=np.ascontiguousarray(g128), cs=np.ascontiguousarray(cs))


def _pk(v, n):
    return np.ascontiguousarray(np.asarray(v, np.float32).reshape(n, 128).T)


def _rep(v):
    v = np.asarray(v, np.float32).reshape(1, -1)
    return np.ascontiguousarray(np.broadcast_to(v, (128, v.shape[1])))


def _shared_inputs(L, norm1_w, w_in, conv_w, conv_b, dt_bias, a_log, d_skip, ssd_norm_w,
                   ret_norm_w, w_out, norm2_w, w_gate, w_up, w_down, final_norm_w):
    m = _host_consts(L)
    cw = np.asarray(conv_w[0], np.float32)
    m.update(
        w_in=np.ascontiguousarray(w_in[0], np.float32), w_out=np.ascontiguousarray(w_out[0], np.float32),
        w_gate=np.ascontiguousarray(w_gate[0], np.float32), w_up=np.ascontiguousarray(w_up[0], np.float32),
        w_down=np.ascontiguousarray(w_down[0], np.float32),
        n1w=_pk(norm1_w[0], 8), n2w=_pk(norm2_w[0], 8),
        mixw=_pk(np.concatenate([np.asarray(ssd_norm_w[0]), np.asarray(ret_norm_w[0])]), 16),
        fw=_rep(final_norm_w),
        cw=np.ascontiguousarray(cw.reshape(4, 12, 128).transpose(2, 1, 0).reshape(128, 48)),
        cb=_pk(conv_b[0], 12), cbrow=np.ascontiguousarray(np.asarray(conv_b[0], np.float32).reshape(1, 1536)),
        dtb=_rep(dt_bias[0]), alog=_rep(a_log[0]), dsk=_rep(d_skip[0]),
    )
    return m


P1_KEYS = ["x", "w_in", "n1w", "cw", "cb", "cbrow", "dtb", "alog", "dsk", "ident", "U", "Ls", "dq", "ks",
           "g128", "cs"]
P2_KEYS = ["x", "w_out", "w_gate", "w_up", "w_down", "n2w", "mixw", "fw", "ident"]
FUSED = True
TRACE = False


def run(x, params, L, ncores):
    shared = _shared_inputs(L, **params)
    xs = [np.ascontiguousarray(np.asarray(x[i, :L], np.float32)) for i in range(ncores)]
    cores = list(range(ncores))
    if FUSED:
        nc = _build(L)
        in_maps = [dict(shared, x=xs[i]) for i in range(ncores)]
        if TRACE:
            res = run_bass_kernel_spmd(nc, in_maps, core_ids=cores, trace=True)
            print("EXEC_NS", res.exec_time_ns, flush=True)
        else:
            res = run_bass_kernel_spmd(nc, in_maps, core_ids=cores)
    else:
        nc1 = _build(L, part=1)
        in1 = [dict({k: shared[k] for k in P1_KEYS if k != "x"}, x=xs[i]) for i in range(ncores)]
        r1 = run_bass_kernel_spmd(nc1, in1, core_ids=cores)
        nc2 = _build(L, part=2)
        in2 = [dict({k: shared[k] for k in P2_KEYS if k != "x"}, x=xs[i], mx=np.asarray(r1.results[i]["mx"]))
               for i in range(ncores)]
        res = run_bass_kernel_spmd(nc2, in2, core_ids=cores)
    return np.stack([np.asarray(r["out"], np.float32) for r in res.results], axis=0)


def kernel(x, norm1_w, w_in, conv_w, conv_b, dt_bias, a_log, d_skip, ssd_norm_w, ret_norm_w,
           w_out, norm2_w, w_gate, w_up, w_down, final_norm_w):
    params = dict(norm1_w=norm1_w, w_in=w_in, conv_w=conv_w, conv_b=conv_b, dt_bias=dt_bias, a_log=a_log,
                  d_skip=d_skip, ssd_norm_w=ssd_norm_w, ret_norm_w=ret_norm_w, w_out=w_out, norm2_w=norm2_w,
                  w_gate=w_gate, w_up=w_up, w_down=w_down, final_norm_w=final_norm_w)
    params = {k: np.asarray(v) for k, v in params.items()}
    return run(np.asarray(x), params, SEQ, NCORES)
```
